# Optimizing a Trainium2 kernel written in Bass

```python
import math
import jax, jax.numpy as jnp
from jax import lax
import numpy as np

D_MODEL = 1024
BATCH = 2
SEQ = 16384
DEPTH = 2

A_HEADS = 4
A_HEAD_DIM = 256
A_WIDTH = A_HEADS * A_HEAD_DIM
A_CHUNK = 64
CONV_WIDTH = 4
B_GROUPS = 4
B_GROUP_DIM = 256
B_WIDTH = B_GROUPS * B_GROUP_DIM
POOL_WINDOWS = (2, 4, 8, 16)
C_HEADS = 16
C_HEAD_DIM = 64
C_WIDTH = C_HEADS * C_HEAD_DIM
KV_RANK = 256
IDX_HEADS = 4
IDX_DIM = 64
TOP_K_MAX = 256
Q_BLOCK = 128
NUM_BUCKETS = 32
MAX_DISTANCE = 128
EPS = 1e-6

N_EVEN = (DEPTH + 1) // 2
N_ODD = DEPTH // 2
EVEN_SPLITS = (A_WIDTH, A_WIDTH, A_WIDTH, A_WIDTH, A_WIDTH, 2 * A_HEADS, B_WIDTH, B_WIDTH)
EVEN_IN = sum(EVEN_SPLITS)
ODD_SPLITS = (C_WIDTH, KV_RANK, IDX_HEADS * IDX_DIM, IDX_DIM, IDX_HEADS, C_WIDTH)
ODD_IN = sum(ODD_SPLITS)

kernel_name = 'hybrid_mlstm_pool_dsa_gated'


def _split(p, sizes):
    pts = [int(v) for v in np.cumsum(sizes)[:-1]]
    return jnp.split(p, pts, axis=-1)


def rmsnorm(x, g):
    xf = x.astype(jnp.float32)
    y = xf * lax.rsqrt(jnp.mean(xf * xf, axis=-1, keepdims=True) + EPS)
    return (y * g.astype(jnp.float32)).astype(x.dtype)


def causal_conv(u, w):
    n_taps = w.shape[0]
    s_len = u.shape[1]
    up = jnp.pad(u, ((0, 0), (n_taps - 1, 0), (0, 0)))
    out = w[0] * up[:, 0:s_len]
    for j in range(1, n_taps):
        out = out + w[j] * up[:, j:j + s_len]
    return out


def mlstm_chunkwise(q, k, v, i_pre, f_pre):
    bsz, s_len, nh, dh = q.shape
    nc = s_len // A_CHUNK
    f32 = jnp.float32

    def to_chunks(a):
        return a.astype(f32).reshape(bsz, nc, A_CHUNK, nh, -1).transpose(1, 0, 3, 2, 4)

    def gate_chunks(a):
        return a.astype(f32).reshape(bsz, nc, A_CHUNK, nh).transpose(1, 0, 3, 2)

    qc = to_chunks(q)
    kc = to_chunks(k) * (dh ** -0.5)
    vc = to_chunks(v)
    ic = gate_chunks(i_pre)
    fc = gate_chunks(f_pre)
    causal = jnp.tril(jnp.ones((A_CHUNK, A_CHUNK), dtype=bool))

    def step(carry, inp):
        c_mat, n_vec, m_prev = carry
        qb, kb, vb, ib, fb = inp
        b = jnp.cumsum(jax.nn.log_sigmoid(fb), axis=-1)
        dmat = jnp.where(causal, b[..., :, None] - b[..., None, :] + ib[..., None, :], -jnp.inf)
        inter = b + m_prev[..., None]
        m_t = jnp.maximum(inter, jnp.max(dmat, axis=-1))
        w = jnp.exp(dmat - m_t[..., None]) * jnp.einsum('bhtd,bhsd->bhts', qb, kb)
        a_inter = jnp.exp(inter - m_t)
        num = a_inter[..., None] * jnp.einsum('bhvk,bhtk->bhtv', c_mat, qb) + jnp.einsum('bhts,bhsv->bhtv', w, vb)
        nq = a_inter * jnp.einsum('bhk,bhtk->bht', n_vec, qb) + jnp.sum(w, axis=-1)
        den = jnp.maximum(jnp.abs(nq), jnp.exp(-m_t))
        h = num / den[..., None]
        b_last = b[..., -1]
        g = b_last[..., None] - b + ib
        m_new = jnp.maximum(b_last + m_prev, jnp.max(g, axis=-1))
        decay = jnp.exp(b_last + m_prev - m_new)
        sw = jnp.exp(g - m_new[..., None])
        c_new = decay[..., None, None] * c_mat + jnp.einsum('bhs,bhsv,bhsk->bhvk', sw, vb, kb)
        n_new = decay[..., None] * n_vec + jnp.einsum('bhs,bhsk->bhk', sw, kb)
        return (c_new, n_new, m_new), h

    init = (jnp.zeros((bsz, nh, dh, dh), f32), jnp.zeros((bsz, nh, dh), f32), jnp.zeros((bsz, nh), f32))
    _, hs = lax.scan(step, init, (qc, kc, vc, ic, fc))
    return hs.transpose(1, 0, 3, 2, 4).reshape(bsz, s_len, nh, dh)


def multiscale_pool(u):
    bsz, s_len, _ = u.shape
    uf = u.astype(jnp.float32)
    cs = jnp.concatenate([jnp.zeros((bsz, 1, B_WIDTH), jnp.float32), jnp.cumsum(uf, axis=1)], axis=1)
    pos = jnp.arange(s_len)
    outs = []
    for gi, win in enumerate(POOL_WINDOWS):
        sl = slice(gi * B_GROUP_DIM, (gi + 1) * B_GROUP_DIM)
        cg = cs[:, :, sl]
        hi = cg[:, 1:]
        lo = jnp.concatenate([jnp.zeros((bsz, win - 1, B_GROUP_DIM), jnp.float32), cg[:, :s_len - win + 1]], axis=1)
        cnt = jnp.minimum(pos + 1, win).astype(jnp.float32)
        outs.append((hi - lo) / cnt[None, :, None] - uf[:, :, sl])
    return jnp.stack(outs, axis=2)


def t5_bucket(rel):
    rel = jnp.maximum(rel, 0)
    max_exact = NUM_BUCKETS // 2
    rel_f = jnp.maximum(rel, 1).astype(jnp.float32)
    large = max_exact + (jnp.log(rel_f / max_exact) / math.log(MAX_DISTANCE / max_exact)
                         * (NUM_BUCKETS - max_exact)).astype(jnp.int32)
    large = jnp.minimum(large, NUM_BUCKETS - 1)
    return jnp.where(rel < max_exact, rel, large)


def even_layer(x, g_norm, w_in, conv_w, b_if, g_head, w_pool, s_pool, w_out):
    bsz, s_len, _ = x.shape
    xn = rmsnorm(x, g_norm)
    p = xn @ w_in
    q, k, v, o, z_a, gates, u, z_b = _split(p, EVEN_SPLITS)
    qk = jax.nn.silu(causal_conv(jnp.concatenate([q, k], axis=-1), conv_w))
    q, k = qk[..., :A_WIDTH], qk[..., A_WIDTH:]
    gates = gates + b_if
    i_pre, f_pre = gates[..., :A_HEADS], gates[..., A_HEADS:]
    hd = (bsz, s_len, A_HEADS, A_HEAD_DIM)
    h = mlstm_chunkwise(q.reshape(hd), k.reshape(hd), v.reshape(hd), i_pre, f_pre)
    h = jax.nn.sigmoid(o.astype(jnp.float32)).reshape(hd) * h
    h = rmsnorm(h, g_head).reshape(bsz, s_len, A_WIDTH).astype(x.dtype)
    y_a = h * jax.nn.silu(z_a)
    pooled = multiscale_pool(u).astype(x.dtype)
    y_b = jnp.einsum('bsgc,gcd->bsgd', pooled, w_pool).reshape(bsz, s_len, B_WIDTH)
    y_b = y_b * s_pool * jax.nn.silu(z_b)
    return x + jnp.concatenate([y_a, y_b], axis=-1) @ w_out


def odd_layer(x, g_norm, w_in, g_kv, w_uk, w_uv, g_q, g_k, w_out, rel_bias):
    bsz, s_len, _ = x.shape
    xn = rmsnorm(x, g_norm)
    p = xn @ w_in
    q, ckv, qi, ki, wi, z = _split(p, ODD_SPLITS)
    hd = (bsz, s_len, C_HEADS, C_HEAD_DIM)
    q = rmsnorm(q.reshape(hd), g_q)
    ckv = rmsnorm(ckv, g_kv)
    k = rmsnorm((ckv @ w_uk).reshape(hd), g_k)
    v = (ckv @ w_uv).reshape(hd)
    qi = qi.reshape(bsz, s_len, IDX_HEADS, IDX_DIM).astype(jnp.float32) * (IDX_DIM ** -0.5)
    ki = ki.astype(jnp.float32)
    wi = wi.astype(jnp.float32) * (IDX_HEADS ** -0.5)
    top_k = min(TOP_K_MAX, s_len // 4)
    n_blocks = s_len // Q_BLOCK
    key_pos = jnp.arange(s_len)

    def block(start):
        qb = lax.dynamic_slice_in_dim(q, start, Q_BLOCK, axis=1)
        qib = lax.dynamic_slice_in_dim(qi, start, Q_BLOCK, axis=1)
        wib = lax.dynamic_slice_in_dim(wi, start, Q_BLOCK, axis=1)
        t = start + jnp.arange(Q_BLOCK)
        score = jnp.einsum('bqh,bqhs->bqs', wib, jax.nn.relu(jnp.einsum('bqhd,bsd->bqhs', qib, ki)))
        score = jnp.where((key_pos[None, :] <= t[:, None])[None], score, -jnp.inf)
        _, idx = lax.top_k(score, top_k)
        valid = idx <= t[None, :, None]
        ksel = jax.vmap(lambda kk, ii: kk[ii])(k, idx)
        vsel = jax.vmap(lambda vv, ii: vv[ii])(v, idx)
        logits = jnp.einsum('bqhd,bqkhd->bqhk', qb, ksel).astype(jnp.float32) * (C_HEAD_DIM ** -0.5)
        bias = rel_bias[t5_bucket(t[None, :, None] - idx)].astype(jnp.float32)
        logits = logits + bias.transpose(0, 1, 3, 2)
        logits = jnp.where(valid[:, :, None, :], logits, -jnp.inf)
        probs = jax.nn.softmax(logits, axis=-1).astype(v.dtype)
        return jnp.einsum('bqhk,bqkhd->bqhd', probs, vsel)

    out = lax.map(block, jnp.arange(n_blocks) * Q_BLOCK)
    out = out.transpose(1, 0, 2, 3, 4).reshape(bsz, s_len, C_WIDTH)
    return x + (out * jax.nn.silu(z)) @ w_out


def setup_inputs(seed: int = 0) -> dict:
    key = jax.random.key(seed)
    ks = jax.random.split(key, 20)
    nrm = jax.random.normal
    f32 = jnp.float32
    b_i = 0.1 * nrm(ks[4], (N_EVEN, A_HEADS), f32)
    b_f = jnp.linspace(3.0, 6.0, A_HEADS, dtype=f32)[None] + 0.1 * nrm(ks[5], (N_EVEN, A_HEADS), f32)
    return {
        'x': nrm(ks[0], (BATCH, SEQ, D_MODEL), f32),
        'e_norm': 1.0 + 0.02 * nrm(ks[1], (N_EVEN, D_MODEL), f32),
        'e_w_in': nrm(ks[2], (N_EVEN, D_MODEL, EVEN_IN), f32) * D_MODEL ** -0.5,
        'e_conv': nrm(ks[3], (N_EVEN, CONV_WIDTH, 2 * A_WIDTH), f32) * CONV_WIDTH ** -0.5,
        'e_b_if': jnp.concatenate([b_i, b_f], axis=-1),
        'e_g_head': 1.0 + 0.02 * nrm(ks[6], (N_EVEN, A_HEADS, A_HEAD_DIM), f32),
        'e_w_pool': nrm(ks[7], (N_EVEN, B_GROUPS, B_GROUP_DIM, B_GROUP_DIM), f32) * B_GROUP_DIM ** -0.5,
        'e_s_pool': 1.0 + 0.02 * nrm(ks[8], (N_EVEN, B_WIDTH), f32),
        'e_w_out': nrm(ks[9], (N_EVEN, A_WIDTH + B_WIDTH, D_MODEL), f32) * (A_WIDTH + B_WIDTH) ** -0.5,
        'o_norm': 1.0 + 0.02 * nrm(ks[10], (N_ODD, D_MODEL), f32),
        'o_w_in': nrm(ks[11], (N_ODD, D_MODEL, ODD_IN), f32) * D_MODEL ** -0.5,
        'o_g_kv': 1.0 + 0.02 * nrm(ks[12], (N_ODD, KV_RANK), f32),
        'o_w_uk': nrm(ks[13], (N_ODD, KV_RANK, C_WIDTH), f32) * KV_RANK ** -0.5,
        'o_w_uv': nrm(ks[14], (N_ODD, KV_RANK, C_WIDTH), f32) * KV_RANK ** -0.5,
        'o_g_q': 1.0 + 0.02 * nrm(ks[15], (N_ODD, C_HEAD_DIM), f32),
        'o_g_k': 1.0 + 0.02 * nrm(ks[16], (N_ODD, C_HEAD_DIM), f32),
        'o_w_out': nrm(ks[17], (N_ODD, C_WIDTH, D_MODEL), f32) * C_WIDTH ** -0.5,
        'rel_bias': 0.5 * nrm(ks[18], (NUM_BUCKETS, C_HEADS), f32),
    }


def reference(x, e_norm, e_w_in, e_conv, e_b_if, e_g_head, e_w_pool, e_s_pool, e_w_out,
              o_norm, o_w_in, o_g_kv, o_w_uk, o_w_uv, o_g_q, o_g_k, o_w_out, rel_bias):
    h = x
    for layer in range(DEPTH):
        j = layer // 2
        if layer % 2 == 0:
            h = even_layer(h, e_norm[j], e_w_in[j], e_conv[j], e_b_if[j], e_g_head[j],
                           e_w_pool[j], e_s_pool[j], e_w_out[j])
        else:
            h = odd_layer(h, o_norm[j], o_w_in[j], o_g_kv[j], o_w_uk[j], o_w_uv[j],
                          o_g_q[j], o_g_k[j], o_w_out[j], rel_bias)
    return h
```

```python
import math
from contextlib import ExitStack

import numpy as np
import ml_dtypes
import concourse.bass as bass
import concourse.mybir as mybir
from concourse.bass_utils import run_bass_kernel_spmd

F32 = mybir.dt.float32
BF16 = mybir.dt.bfloat16
AF = mybir.ActivationFunctionType
ALU = mybir.AluOpType
AX = mybir.AxisListType
NPBF = ml_dtypes.bfloat16

ENGS = ("pe", "act", "dve", "pool", "sp")
NSLOT = 8
EPS = 1e-6


class Buf:
    __slots__ = ("name", "w", "r")

    def __init__(self, name=""):
        self.name = name
        self.w = None
        self.r = []


class Sched:
    def __init__(self, nc, same_engine_sync=True):
        self.nc = nc
        self.same_engine_sync = same_engine_sync
        self.ops = {e: [] for e in ENGS}
        self.comp = {e: [] for e in ENGS}
        self.dma_rr = {e: 0 for e in ENGS}
        for e in ENGS:
            for s in range(NSLOT):
                self.comp["dma_%s_%d" % (e, s)] = []
        self.seen = {e: {} for e in ENGS}

    def op(self, eng, fn, reads=(), writes=(), dma=False):
        if dma:
            slot = self.dma_rr[eng]
            self.dma_rr[eng] = (slot + 1) % NSLOT
            ckey = "dma_%s_%d" % (eng, slot)
        else:
            ckey = eng
        rec = {"fn": fn, "waits": [], "need_inc": bool(dma), "ckey": ckey}
        my_idx = len(self.comp[ckey])
        best = {}

        def add(dep):
            k, i = dep
            if k == eng and (eng == "pe" or not self.same_engine_sync):
                return
            if i > best.get(k, -1):
                best[k] = i

        for b in reads:
            if b.w is not None:
                add(b.w)
        for b in writes:
            if b.w is not None:
                add(b.w)
            for d in b.r:
                add(d)
        if dma and my_idx > 0:
            add((ckey, my_idx - 1))
        seen = self.seen[eng]
        for k, i in best.items():
            if seen.get(k, -1) >= i:
                continue
            seen[k] = i
            rec["waits"].append((k, i))
            self.comp[k][i]["need_inc"] = True
        self.ops[eng].append(rec)
        self.comp[ckey].append(rec)
        me = (ckey, my_idx)
        for b in reads:
            b.r.append(me)
        for b in writes:
            b.w = me
            b.r = []
        return me

    def final_waits(self, eng, bufs):
        rec = {"fn": None, "waits": [], "need_inc": False, "ckey": eng}
        for b in bufs:
            if b.w is not None:
                k, i = b.w
                rec["waits"].append((k, i))
                self.comp[k][i]["need_inc"] = True
        self.ops[eng].append(rec)

    def stream_keys(self):
        return [k for k, l in self.comp.items() if any(r["need_inc"] for r in l)]

    def emit(self, block, sems):
        for k, lst in self.comp.items():
            c = 0
            step = 16 if k.startswith("dma_") else 1
            for rec in lst:
                if rec["need_inc"]:
                    c += step
                rec["cnt"] = c
        comp = self.comp

        def make(eng_name):
            ops = self.ops[eng_name]

            def body(e):
                for rec in ops:
                    for (k, i) in rec["waits"]:
                        e.wait_ge(sems[k], comp[k][i]["cnt"])
                    if rec["fn"] is None:
                        continue
                    ins = rec["fn"](e)
                    if rec["need_inc"]:
                        ins.then_inc(sems[rec["ckey"]], 16 if rec["ckey"].startswith("dma_") else 1)
            return body

        if self.ops["pe"]:
            block.tensor(make("pe"))
        if self.ops["act"]:
            block.scalar(make("act"))
        if self.ops["dve"]:
            block.vector(make("dve"))
        if self.ops["pool"]:
            block.gpsimd(make("pool"))
        if self.ops["sp"]:
            block.sync(make("sp"))

    def finish(self, es, out_bufs):
        self.final_waits("sp", out_bufs)
        sems = {k: es.enter_context(self.nc.semaphore("s_" + k)) for k in self.stream_keys()}
        with self.nc.Block() as block:
            self.emit(block, sems)


class Ctx:
    def __init__(self, nc, es):
        self.nc = nc
        self.es = es
        self.S = Sched(nc)
        self.n = 0

    def sb(self, shape, dt, name=None):
        self.n += 1
        t = self.es.enter_context(self.nc.sbuf_tensor("%s_%d" % (name or "t", self.n), list(shape), dt))
        return t, Buf(name or "t")

    def bank(self, name=None):
        self.n += 1
        t = self.es.enter_context(self.nc.psum_tensor("%s_%d" % (name or "p", self.n), [128, 512], F32))
        return t

    def dram_in(self, name, shape, dt=F32):
        return self.nc.dram_tensor(name, list(shape), dt, kind="ExternalInput").ap()

    def dram_out(self, name, shape, dt=F32):
        return self.nc.dram_tensor(name, list(shape), dt, kind="ExternalOutput").ap()

    def load(self, tile_ap, dram_ap, buf, eng="sp"):
        self.S.op(eng, lambda e: e.dma_start(out=tile_ap, in_=dram_ap), writes=[buf], dma=True)


L1_NCOL = 1794
LN16 = math.log(16.0)


def build_L1(S_len):
    NCH = S_len // 128
    nc = bass.Bass("TRN2", target_bir_lowering=False)
    es = ExitStack()
    with es:
        C = Ctx(nc, es)
        S = C.S
        x_d = C.dram_in("x", [S_len, 1024])
        w_d = C.dram_in("w", [1024, L1_NCOL])
        gn_d = C.dram_in("gn", [128, 8])
        convw_d = C.dram_in("convw", [128, 16])
        bif_d = C.dram_in("bif", [128, 2])
        ghead_d = C.dram_in("ghead", [128, 256])
        wpool_d = C.dram_in("wpool", [256, 256])
        spool_d = C.dram_in("spool", [128, 256])
        cst_d = C.dram_in("cst", [128, 6 * 128])
        y_d = C.dram_out("y", [S_len, 512], BF16)
        ybuf = Buf("y")

        cst, cstB = C.sb([128, 6, 128], F32, "cst")
        C.load(cst[:], cst_d.rearrange("p (a b) -> p a b", a=6), cstB)
        ident32, tri32, maskT32, acur0, acur, aprev = [cst[:, i, :] for i in range(6)]
        id16, id16B = C.sb([128, 128], BF16, "id16")
        S.op("dve", lambda e: e.tensor_copy(out=id16[:], in_=ident32), reads=[cstB], writes=[id16B])
        ones32, onesB = C.sb([128, 128], F32, "ones")
        S.op("pool", lambda e: e.memset(ones32[:], 1.0), writes=[onesB])
        gn, gnB = C.sb([128, 8], F32, "gn")
        C.load(gn[:], gn_d, gnB)
        convw, convwB = C.sb([128, 16], F32, "convw")
        C.load(convw[:], convw_d, convwB)
        bif, bifB = C.sb([128, 2], F32, "bif")
        C.load(bif[:], bif_d, bifB)
        ghead, gheadB = C.sb([128, 256], F32, "ghead")
        C.load(ghead[:], ghead_d, gheadB)
        spool, spoolB = C.sb([128, 256], F32, "spool")
        C.load(spool[:], spool_d, spoolB)
        wp32, wp32B = C.sb([128, 2, 256], F32, "wp32")
        C.load(wp32[:], wpool_d.rearrange("(k p) n -> p k n", p=128), wp32B)
        wp16, wp16B = C.sb([128, 2, 256], BF16, "wp16")
        S.op("dve", lambda e: e.tensor_copy(out=wp16[:], in_=wp32[:]), reads=[wp32B], writes=[wp16B])
        w16, w16B = C.sb([128, 8, L1_NCOL], BF16, "w16")
        wst, wstB = C.sb([128, L1_NCOL], F32, "wst")
        for kc in range(8):
            C.load(wst[:], w_d[kc * 128:(kc + 1) * 128, :], wstB)
            S.op("dve", lambda e, kc=kc: e.tensor_scalar(out=w16[:, kc, :], in0=wst[:], scalar1=gn[:, kc:kc + 1],
                                                         scalar2=None, op0=ALU.mult),
                 reads=[wstB, gnB], writes=[w16B])

        c32, c32B = C.sb([128, 2, 257], F32, "c32")
        c16, c16B = C.sb([128, 2, 257], BF16, "c16")
        S.op("pool", lambda e: e.memset(c32[:], 0.0), writes=[c32B])
        S.op("pool", lambda e: e.memset(c16[:], 0.0), writes=[c16B])

        def dbl(shape, dt, name):
            a = [C.sb(shape, dt, name + str(i)) for i in range(2)]
            return [t for t, _ in a], [b for _, b in a]
        xt, xtB = dbl([128, 1024], F32, "xt")
        xn16, xn16B = dbl([128, 1024], BF16, "xn16")
        xnT, xnTB = dbl([128, 8, 128], BF16, "xnT")
        pre, preB = dbl([128, 4, 131], F32, "pre")
        cq, cqB = dbl([128, 4, 128], F32, "cq")
        qk16, qk16B = dbl([128, 4, 128], BF16, "qk16")
        vaug, vaugB = dbl([128, 257], BF16, "vaug")
        u32, u32B = dbl([128, 256], F32, "u32")
        sigo, sigoB = dbl([128, 256], F32, "sigo")
        szA, szAB = dbl([128, 256], F32, "szA")
        szB, szBB = dbl([128, 256], F32, "szB")
        gates, gatesB = dbl([128, 2], F32, "gates")
        sml, smlB = dbl([128, 16], F32, "sml")
        wpT, wpTB = dbl([128, 128], BF16, "wpT")
        ktl, ktlB = dbl([128, 256], BF16, "ktl")
        hs, hsB = dbl([128, 256], F32, "hs")
        junk, junkB = dbl([128, 1024], BF16, "junk")
        pT16, pT16B = dbl([128, 2, 128], BF16, "pT16")
        yout, youtB = dbl([128, 512], BF16, "yout")
        for j in range(2):
            S.op("pool", lambda e, j=j: e.memset(vaug[j][:, 256:257], 1.0), writes=[vaugB[j]])
            S.op("pool", lambda e, j=j: e.memset(pre[j][:], 0.0), writes=[preB[j]])
            S.op("pool", lambda e, j=j: e.memset(u32[j][:], 0.0), writes=[u32B[j]])

        P1, P2, P3, P4, P5, P6, P7, P8 = [C.bank("P%d" % i) for i in range(8)]
        P1a = P1[:, 0:256].bitcast(BF16).rearrange("p (a b) -> p a b", a=4)
        P1aB = Buf("P1a")
        P1b = P1[:, 256:512]
        P1bB = Buf("P1b")
        P2v = P2[:, :].rearrange("p (a b) -> p a b", a=4)
        P2B = Buf("P2")
        P3B, P4B, P5B = Buf("P3"), Buf("P4"), Buf("P5")
        Pg = P5[:, 260:262]
        PgB = Buf("Pg")
        PsT = P6[:, 0:128]
        PsTB = Buf("PsT")
        PpT = P6[:, 128:384].rearrange("p (a b) -> p a b", a=2)
        PpTB = Buf("PpT")
        PkT = P6[:, 384:512].bitcast(BF16).rearrange("p (a b) -> p a b", a=2)
        PkTB = Buf("PkT")
        Pnum = P7[:, 0:257]
        PnumB = Buf("Pnum")
        Pdc = P8[:, 0:257]
        PdcB = Buf("Pdc")

        for c in range(NCH):
            j = c % 2
            jp = 1 - j
            C.load(xt[j][:], x_d[c * 128:(c + 1) * 128, :], xtB[j])
            S.op("act", lambda e, j=j: e.activation(out=junk[j][:], in_=xt[j][:], func=AF.Square,
                                                    accum_out=sml[j][:, 0:1]),
                 reads=[xtB[j]], writes=[junkB[j], smlB[j]])
            S.op("act", lambda e, j=j: e.activation(out=sml[j][:, 1:2], in_=sml[j][:, 0:1], func=AF.Ln,
                                                    scale=1.0 / 1024, bias=EPS),
                 reads=[smlB[j]], writes=[smlB[j]])
            S.op("act", lambda e, j=j: e.activation(out=sml[j][:, 2:3], in_=sml[j][:, 1:2], func=AF.Exp, scale=-0.5),
                 reads=[smlB[j]], writes=[smlB[j]])
            S.op("dve", lambda e, j=j: e.tensor_scalar(out=xn16[j][:], in0=xt[j][:], scalar1=sml[j][:, 2:3],
                                                       scalar2=None, op0=ALU.mult),
                 reads=[xtB[j], smlB[j]], writes=[xn16B[j]])
            for r in range(2):
                for k in range(4):
                    S.op("pe", lambda e, j=j, r=r, k=k: e.transpose(out=P1a[:, k, :],
                                                                   in_=xn16[j][:, (r * 4 + k) * 128:(r * 4 + k + 1) * 128],
                                                                   identity=id16[:]),
                         reads=[xn16B[j], id16B], writes=[P1aB])
                S.op("act", lambda e, j=j, r=r: e.activation(out=xnT[j][:, r * 4:(r + 1) * 4, :], in_=P1a, func=AF.Copy),
                     reads=[P1aB], writes=[xnTB[j]])
            for blk in range(4):
                for kc in range(8):
                    S.op("pe", lambda e, j=j, blk=blk, kc=kc: e.matmul(P2v[:, blk, :], lhsT=w16[:, kc, blk * 128:(blk + 1) * 128],
                                                                      rhs=xnT[j][:, kc, :], start=(kc == 0), stop=(kc == 7)),
                         reads=[w16B, xnTB[j]], writes=[P2B])
            for (Pb, PbB, c0, ncol) in ((P3, P3B, 512, 512), (P4, P4B, 1024, 512), (P5, P5B, 1536, 258)):
                for kc in range(8):
                    S.op("pe", lambda e, j=j, Pb=Pb, c0=c0, ncol=ncol, kc=kc: e.matmul(
                        Pb[:, 0:ncol], lhsT=xnT[j][:, kc, :], rhs=w16[:, kc, c0:c0 + ncol], start=(kc == 0), stop=(kc == 7)),
                        reads=[w16B, xnTB[j]], writes=[PbB])
            S.op("pool", lambda e, j=j, jp=jp: e.tensor_copy(out=pre[j][:, :, 0:3], in_=pre[jp][:, :, 128:131]),
                 reads=[preB[jp]], writes=[preB[j]])
            S.op("act", lambda e, j=j: e.activation(out=pre[j][:, :, 3:131], in_=P2v, func=AF.Copy),
                 reads=[P2B], writes=[preB[j]])
            S.op("act", lambda e, j=j: e.activation(out=vaug[j][:, 0:256], in_=P3[:, 0:256], func=AF.Copy),
                 reads=[P3B], writes=[vaugB[j]])
            S.op("act", lambda e, j=j: e.activation(out=u32[j][:], in_=P3[:, 256:512], func=AF.Copy),
                 reads=[P3B], writes=[u32B[j]])
            S.op("act", lambda e, j=j: e.activation(out=sigo[j][:], in_=P4[:, 0:256], func=AF.Sigmoid),
                 reads=[P4B], writes=[sigoB[j]])
            S.op("act", lambda e, j=j: e.activation(out=szA[j][:], in_=P4[:, 256:512], func=AF.Silu),
                 reads=[P4B], writes=[szAB[j]])
            S.op("act", lambda e, j=j: e.activation(out=szB[j][:], in_=P5[:, 0:256], func=AF.Silu),
                 reads=[P5B], writes=[szBB[j]])
            S.op("dve", lambda e, j=j: e.tensor_tensor(out=gates[j][:], in0=P5[:, 256:258], in1=bif[:], op=ALU.add),
                 reads=[P5B, bifB], writes=[gatesB[j]])
            for blk in range(4):
                eng = "dve"
                S.op(eng, lambda e, j=j, blk=blk: e.tensor_scalar(out=cq[j][:, blk, :], in0=pre[j][:, blk, 0:128],
                                                                  scalar1=convw[:, blk * 4:blk * 4 + 1], scalar2=None, op0=ALU.mult),
                     reads=[preB[j], convwB], writes=[cqB[j]])
                for tap in range(1, 4):
                    S.op(eng, lambda e, j=j, blk=blk, tap=tap: e.scalar_tensor_tensor(
                        out=cq[j][:, blk, :], in0=pre[j][:, blk, tap:tap + 128], scalar=convw[:, blk * 4 + tap:blk * 4 + tap + 1],
                        in1=cq[j][:, blk, :], op0=ALU.mult, op1=ALU.add),
                        reads=[preB[j], convwB, cqB[j]], writes=[cqB[j]])
            S.op("act", lambda e, j=j: e.activation(out=qk16[j][:], in_=cq[j][:], func=AF.Silu),
                 reads=[cqB[j]], writes=[qk16B[j]])
            S.op("act", lambda e, j=j: e.activation(out=sml[j][:, 3:4], in_=gates[j][:, 1:2], func=AF.Exp, scale=-1.0),
                 reads=[gatesB[j]], writes=[smlB[j]])
            S.op("act", lambda e, j=j: e.activation(out=sml[j][:, 4:5], in_=sml[j][:, 3:4], func=AF.Ln, bias=1.0),
                 reads=[smlB[j]], writes=[smlB[j]])
            S.op("pe", lambda e, j=j: e.matmul(Pg[:, 0:1], lhsT=tri32, rhs=sml[j][:, 4:5], start=True, stop=True),
                 reads=[smlB[j], cstB], writes=[PgB])
            S.op("pe", lambda e, j=j: e.matmul(Pg[:, 1:2], lhsT=ones32[:], rhs=sml[j][:, 4:5], start=True, stop=True),
                 reads=[smlB[j], onesB], writes=[PgB])
            S.op("dve", lambda e, j=j: e.scalar_tensor_tensor(out=sml[j][:, 5:6], in0=gates[j][:, 0:1], scalar=-LN16,
                                                              in1=Pg[:, 0:1], op0=ALU.add, op1=ALU.add),
                 reads=[gatesB[j], PgB], writes=[smlB[j]])
            S.op("dve", lambda e, j=j: e.tensor_tensor(out=sml[j][:, 6:7], in0=sml[j][:, 5:6], in1=Pg[:, 1:2], op=ALU.subtract),
                 reads=[smlB[j], PgB], writes=[smlB[j]])
            S.op("dve", lambda e, j=j: e.tensor_copy(out=sml[j][:, 7:8], in_=Pg[:, 0:1]),
                 reads=[PgB], writes=[smlB[j]])
            S.op("dve", lambda e, j=j: e.tensor_scalar(out=sml[j][:, 8:9], in0=Pg[:, 1:2], scalar1=-1.0, scalar2=None, op0=ALU.mult),
                 reads=[PgB], writes=[smlB[j]])
            S.op("act", lambda e, j=j: e.activation(out=sml[j][:, 9:13], in_=sml[j][:, 5:9], func=AF.Exp),
                 reads=[smlB[j]], writes=[smlB[j]])
            g_s = lambda j: sml[j][:, 9:10]
            ga_s = lambda j: sml[j][:, 10:11]
            inva = lambda j: sml[j][:, 11:12]
            aL = lambda j: sml[j][:, 12:13]
            for hf in range(2):
                S.op("pe", lambda e, j=j, hf=hf: e.matmul(PsT, lhsT=qk16[j][:, 2 + hf, :], rhs=qk16[j][:, hf, :],
                                                          start=(hf == 0), stop=(hf == 1)),
                     reads=[qk16B[j]], writes=[PsTB])
            S.op("dve", lambda e, j=j: e.scalar_tensor_tensor(out=wpT[j][:], in0=PsT, scalar=g_s(j), in1=maskT32,
                                                              op0=ALU.mult, op1=ALU.mult),
                 reads=[PsTB, smlB[j], cstB], writes=[wpTB[j]])
            for hf in range(2):
                S.op("pe", lambda e, j=j, hf=hf: e.transpose(out=PkT[:, hf, :], in_=qk16[j][:, 2 + hf, :], identity=id16[:]),
                     reads=[qk16B[j], id16B], writes=[PkTB])
            S.op("dve", lambda e, j=j: e.tensor_scalar(out=ktl[j][:], in0=PkT.rearrange("p a b -> p (a b)"), scalar1=ga_s(j),
                                                       scalar2=None, op0=ALU.mult),
                 reads=[PkTB, smlB[j]], writes=[ktlB[j]])
            S.op("pe", lambda e, j=j: e.matmul(Pnum, lhsT=wpT[j][:], rhs=vaug[j][:], start=True, stop=False),
                 reads=[wpTB[j], vaugB[j]], writes=[PnumB])
            for hf in range(2):
                S.op("pe", lambda e, j=j, hf=hf: e.matmul(Pnum, lhsT=qk16[j][:, hf, :], rhs=c16[:, hf, :],
                                                          start=False, stop=(hf == 1)),
                     reads=[qk16B[j], c16B], writes=[PnumB])
            for hf in range(2):
                S.op("pe", lambda e, j=j, hf=hf: e.matmul(Pdc, lhsT=ktl[j][:, hf * 128:(hf + 1) * 128], rhs=vaug[j][:],
                                                          start=True, stop=True),
                     reads=[ktlB[j], vaugB[j]], writes=[PdcB])
                S.op("dve", lambda e, j=j, hf=hf: e.scalar_tensor_tensor(out=c16[:, hf, :], in0=c32[:, hf, :], scalar=aL(j),
                                                                         in1=Pdc, op0=ALU.mult, op1=ALU.add),
                     reads=[c32B, smlB[j], PdcB], writes=[c16B])
                S.op("dve", lambda e, j=j, hf=hf: e.scalar_tensor_tensor(out=c32[:, hf, :], in0=c32[:, hf, :], scalar=aL(j),
                                                                         in1=Pdc, op0=ALU.mult, op1=ALU.add),
                     reads=[c32B, smlB[j], PdcB], writes=[c32B])
            S.op("dve", lambda e, j=j: e.tensor_scalar(out=sml[j][:, 13:14], in0=Pnum[:, 256:257], scalar1=-1.0, scalar2=None,
                                                       op0=ALU.mult),
                 reads=[PnumB], writes=[smlB[j]])
            S.op("dve", lambda e, j=j: e.tensor_tensor(out=sml[j][:, 13:14], in0=sml[j][:, 13:14], in1=Pnum[:, 256:257], op=ALU.max),
                 reads=[smlB[j], PnumB], writes=[smlB[j]])
            S.op("dve", lambda e, j=j: e.tensor_tensor(out=sml[j][:, 13:14], in0=sml[j][:, 13:14], in1=inva(j), op=ALU.max),
                 reads=[smlB[j]], writes=[smlB[j]])
            S.op("dve", lambda e, j=j: e.reciprocal(out=sml[j][:, 14:15], in_=sml[j][:, 13:14]),
                 reads=[smlB[j]], writes=[smlB[j]])
            S.op("dve", lambda e, j=j: e.scalar_tensor_tensor(out=hs[j][:], in0=Pnum[:, 0:256], scalar=sml[j][:, 14:15],
                                                              in1=sigo[j][:], op0=ALU.mult, op1=ALU.mult),
                 reads=[PnumB, smlB[j], sigoB[j]], writes=[hsB[j]])
            S.op("act", lambda e, j=j: e.activation(out=junk[j][:, 0:256], in_=hs[j][:], func=AF.Square,
                                                    accum_out=sml[j][:, 15:16]),
                 reads=[hsB[j]], writes=[junkB[j], smlB[j]])
            S.op("act", lambda e, j=j: e.activation(out=sml[j][:, 15:16], in_=sml[j][:, 15:16], func=AF.Ln,
                                                    scale=1.0 / 256, bias=EPS),
                 reads=[smlB[j]], writes=[smlB[j]])
            S.op("act", lambda e, j=j: e.activation(out=sml[j][:, 15:16], in_=sml[j][:, 15:16], func=AF.Exp, scale=-0.5),
                 reads=[smlB[j]], writes=[smlB[j]])
            S.op("dve", lambda e, j=j: e.scalar_tensor_tensor(out=hs[j][:], in0=hs[j][:], scalar=sml[j][:, 15:16],
                                                              in1=ghead[:], op0=ALU.mult, op1=ALU.mult),
                 reads=[hsB[j], smlB[j], gheadB], writes=[hsB[j]])
            S.op("dve", lambda e, j=j: e.tensor_tensor(out=yout[j][:, 0:256], in0=hs[j][:], in1=szA[j][:], op=ALU.mult),
                 reads=[hsB[j], szAB[j]], writes=[youtB[j]])
            for cb in range(2):
                S.op("pe", lambda e, j=j, cb=cb, c=c: e.matmul(PpT[:, cb, :], lhsT=u32[j][:, cb * 128:(cb + 1) * 128],
                                                              rhs=(acur0 if c == 0 else acur), start=True, stop=False),
                     reads=[u32B[j], cstB], writes=[PpTB])
                S.op("pe", lambda e, jp=jp, cb=cb: e.matmul(PpT[:, cb, :], lhsT=u32[jp][64:128, cb * 128:(cb + 1) * 128],
                                                           rhs=aprev[64:128, :], start=False, stop=True),
                     reads=[u32B[jp], cstB], writes=[PpTB])
            S.op("act", lambda e, j=j: e.activation(out=pT16[j][:], in_=PpT, func=AF.Copy),
                 reads=[PpTB], writes=[pT16B[j]])
            for cb in range(2):
                S.op("pe", lambda e, j=j, cb=cb: e.matmul(P1b, lhsT=pT16[j][:, cb, :], rhs=wp16[:, cb, :],
                                                          start=(cb == 0), stop=(cb == 1)),
                     reads=[pT16B[j], wp16B], writes=[P1bB])
            S.op("dve", lambda e, j=j: e.tensor_tensor(out=hs[j][:], in0=P1b, in1=spool[:], op=ALU.mult),
                 reads=[P1bB, spoolB, youtB[j]], writes=[hsB[j]])
            S.op("dve", lambda e, j=j: e.tensor_tensor(out=yout[j][:, 256:512], in0=hs[j][:], in1=szB[j][:], op=ALU.mult),
                 reads=[hsB[j], szBB[j]], writes=[youtB[j]])
            S.op("sp", lambda e, j=j, c=c: e.dma_start(out=y_d[c * 128:(c + 1) * 128, :], in_=yout[j][:]),
                 reads=[youtB[j]], writes=[ybuf], dma=True)
        S.finish(es, [ybuf])
    return nc


POOL_WINDOWS = (2, 4, 8, 16)


def l1_consts(g):
    win = POOL_WINDOWS[g]
    ident = np.eye(128, dtype=np.float32)
    r = np.arange(128)
    tri = (r[:, None] <= r[None, :]).astype(np.float32)
    maskT = tri.copy()
    def amat(first):
        cur = np.zeros((128, 128), np.float32)
        prev = np.zeros((128, 128), np.float32)
        for t in range(128):
            cnt = min(t + 1, win) if first else win
            for jj in range(win):
                tp = t - jj
                if tp >= 0:
                    cur[tp, t] += 1.0 / cnt
                elif not first:
                    prev[128 + tp, t] += 1.0 / cnt
            cur[t, t] -= 1.0
        return cur, prev
    cur0, _ = amat(True)
    cur, prev = amat(False)
    return np.concatenate([ident, tri, maskT, cur0, cur, prev], axis=1)


def run_L1(inp, S_len):
    B = inp["x"].shape[0]
    e_w_in = inp["e_w_in"][0]
    in_maps = []
    for core in range(8):
        b, h = core // 4, core % 4
        if b >= B:
            b = B - 1
        def cs(i0):
            return slice(i0 + h * 256, i0 + (h + 1) * 256)
        cols = np.concatenate([
            np.arange(1024)[cs(0)], 1024 + np.arange(1024)[cs(0)],
            2048 + np.arange(1024)[cs(0)], 5128 + np.arange(1024)[cs(0)],
            3072 + np.arange(1024)[cs(0)], 4096 + np.arange(1024)[cs(0)],
            6152 + np.arange(1024)[cs(0)], np.array([5120 + h, 5124 + h]),
        ])
        w = np.ascontiguousarray(e_w_in[:, cols])
        gn = np.ascontiguousarray(inp["e_norm"][0].reshape(8, 128).T)
        conv = inp["e_conv"][0]
        convw = np.zeros((128, 16), np.float32)
        for blk in range(4):
            base = (h * 256 + blk * 128) if blk < 2 else (1024 + h * 256 + (blk - 2) * 128)
            convw[:, blk * 4:(blk + 1) * 4] = conv[:, base:base + 128].T
        bif = np.tile(np.array([[inp["e_b_if"][0, h], inp["e_b_if"][0, 4 + h]]], np.float32), (128, 1))
        ghead = np.tile(inp["e_g_head"][0, h][None, :], (128, 1)).astype(np.float32)
        spool = np.tile(inp["e_s_pool"][0, h * 256:(h + 1) * 256][None, :], (128, 1)).astype(np.float32)
        in_maps.append({
            "x": np.ascontiguousarray(inp["x"][b, :S_len]),
            "w": w, "gn": gn, "convw": convw, "bif": bif, "ghead": ghead,
            "wpool": np.ascontiguousarray(inp["e_w_pool"][0, h]), "spool": spool,
            "cst": l1_consts(h),
        })
    nc = build_L1(S_len)
    res = run_bass_kernel_spmd(nc, in_maps, core_ids=list(range(8)))
    y = np.zeros((B, S_len, 2048), NPBF)
    for core in range(8):
        b, h = core // 4, core % 4
        if b >= B:
            continue
        yy = np.asarray(res.results[core]["y"]).view(NPBF) if res.results[core]["y"].dtype != NPBF else res.results[core]["y"]
        y[b, :, h * 256:(h + 1) * 256] = yy[:, 0:256]
        y[b, :, 1024 + h * 256:1024 + (h + 1) * 256] = yy[:, 256:512]
    return y


L2_NCOL = 2628
LN8 = math.log(8.0)


def build_L2(NT):
    nc = bass.Bass("TRN2", target_bir_lowering=False)
    es = ExitStack()
    with es:
        C = Ctx(nc, es)
        S = C.S
        x_d = C.dram_in("x", [NT * 128, 1024])
        yT_d = C.dram_in("yT", [NT, 128, 16 * 128], BF16)
        wout_d = C.dram_in("wout", [2048, 1024])
        win_d = C.dram_in("win", [1024, L2_NCOL])
        gn_d = C.dram_in("gn", [128, 8])
        wuk_d = C.dram_in("wuk", [256, 1024])
        wuv_d = C.dram_in("wuv", [256, 1024])
        gkv_d = C.dram_in("gkv", [128, 2])
        gqk_d = C.dram_in("gqk", [128, 128])
        ident_d = C.dram_in("ident", [128, 128])
        h1_d = C.dram_out("h1", [NT * 128, 1024])
        qn_d = C.dram_out("qn", [NT * 128, 1024], BF16)
        kn_d = C.dram_out("kn", [NT * 128, 1024], BF16)
        v_d = C.dram_out("v", [NT * 128, 1024], BF16)
        idx_d = C.dram_out("idx", [NT * 128, 320], BF16)
        wi_d = C.dram_out("wi", [NT * 128, 4])
        sz_d = C.dram_out("sz", [NT * 128, 1024], BF16)
        outB = Buf("outs")

        id32, id32B = C.sb([128, 128], F32, "id32")
        C.load(id32[:], ident_d, id32B)
        id16, id16B = C.sb([128, 128], BF16, "id16")
        S.op("dve", lambda e: e.tensor_copy(out=id16[:], in_=id32[:]), reads=[id32B], writes=[id16B])
        gn, gnB = C.sb([128, 8], F32, "gn")
        C.load(gn[:], gn_d, gnB)
        gkv, gkvB = C.sb([128, 2], F32, "gkv")
        C.load(gkv[:], gkv_d, gkvB)
        gqk, gqkB = C.sb([128, 128], F32, "gqk")
        C.load(gqk[:], gqk_d, gqkB)
        wout16, wout16B = C.sb([128, 16, 1024], BF16, "wout16")
        win16, win16B = C.sb([128, 8, L2_NCOL], BF16, "win16")
        wuk16, wuk16B = C.sb([128, 2, 1024], BF16, "wuk16")
        wuv16, wuv16B = C.sb([128, 2, 1024], BF16, "wuv16")
        wst, wstB = C.sb([128, L2_NCOL], F32, "wst")
        for kc in range(16):
            C.load(wst[:, 0:1024], wout_d[kc * 128:(kc + 1) * 128, :], wstB)
            S.op("dve", lambda e, kc=kc: e.tensor_copy(out=wout16[:, kc, :], in_=wst[:, 0:1024]), reads=[wstB], writes=[wout16B])
        for kc in range(8):
            C.load(wst[:], win_d[kc * 128:(kc + 1) * 128, :], wstB)
            S.op("dve", lambda e, kc=kc: e.tensor_scalar(out=win16[:, kc, :], in0=wst[:], scalar1=gn[:, kc:kc + 1],
                                                         scalar2=None, op0=ALU.mult),
                 reads=[wstB, gnB], writes=[win16B])
        for (wd, w16t, w16tB) in ((wuk_d, wuk16, wuk16B), (wuv_d, wuv16, wuv16B)):
            for kc in range(2):
                C.load(wst[:, 0:1024], wd[kc * 128:(kc + 1) * 128, :], wstB)
                S.op("dve", lambda e, kc=kc, w16t=w16t: e.tensor_scalar(out=w16t[:, kc, :], in0=wst[:, 0:1024], scalar1=gkv[:, kc:kc + 1],
                                                                        scalar2=None, op0=ALU.mult),
                     reads=[wstB, gkvB], writes=[w16tB])

        def dbl(shape, dt, name):
            a = [C.sb(shape, dt, name + str(i)) for i in range(2)]
            return [t for t, _ in a], [b for _, b in a]
        xt, xtB = dbl([128, 1024], F32, "xt")
        yT, yTB = dbl([128, 16, 128], BF16, "yT")
        h1, h1B = dbl([128, 1024], F32, "h1")
        xn16, xn16B = dbl([128, 1024], BF16, "xn16")
        xnT, xnTB = dbl([128, 8, 128], BF16, "xnT")
        junk, junkB = dbl([128, 1024], BF16, "junk")
        sml, smlB = dbl([128, 64], F32, "sml")
        sq, sqB = dbl([128, 1024], F32, "sq")
        qn, qnB = dbl([128, 1024], BF16, "qn")
        kn, knB = dbl([128, 1024], BF16, "kn")
        vv, vvB = dbl([128, 1024], BF16, "vv")
        szt, sztB = dbl([128, 1024], BF16, "szt")
        idx, idxB = dbl([128, 320], BF16, "idx")
        wi, wiB = dbl([128, 4], F32, "wi")
        ckvn, ckvnB = dbl([128, 256], BF16, "ckvn")
        ckvT, ckvTB = dbl([128, 2, 128], BF16, "ckvT")

        PT, A0, A1, B0, B1, PC, PD, PE_ = [C.bank("L2P%d" % i) for i in range(8)]
        PTv = PT[:, :].bitcast(BF16).rearrange("p (a b) -> p a b", a=8)
        PTB, A0B, A1B, B0B, B1B, PCB, PDB = [Buf() for _ in range(7)]

        def rows(t):
            return slice(t * 128, (t + 1) * 128)

        def headnorm(j, srcs, srcBs, gcol, dst, dstB, extra_bias, smc):
            for hb in range(2):
                S.op("act", lambda e, j=j, hb=hb: e.activation(out=sq[j][:, hb * 512:(hb + 1) * 512], in_=srcs[hb][:, :],
                                                               func=AF.Square),
                     reads=[srcBs[hb]], writes=[sqB[j]])
            S.op("dve", lambda e, j=j: e.tensor_reduce(out=sml[j][:, smc:smc + 16], in_=sq[j][:].rearrange("p (h d) -> p h d", d=64),
                                                       axis=AX.X, op=ALU.add),
                 reads=[sqB[j]], writes=[smlB[j]])
            S.op("act", lambda e, j=j: e.activation(out=sml[j][:, smc:smc + 16], in_=sml[j][:, smc:smc + 16], func=AF.Ln,
                                                    scale=1.0 / 64, bias=EPS),
                 reads=[smlB[j]], writes=[smlB[j]])
            S.op("act", lambda e, j=j: e.activation(out=sml[j][:, smc:smc + 16], in_=sml[j][:, smc:smc + 16], func=AF.Exp,
                                                    scale=-0.5, bias=extra_bias),
                 reads=[smlB[j]], writes=[smlB[j]])
            for hb in range(2):
                S.op("dve", lambda e, j=j, hb=hb: e.tensor_tensor(
                    out=sq[j][:, hb * 512:(hb + 1) * 512].rearrange("p (h d) -> p h d", d=64),
                    in0=srcs[hb][:, :].rearrange("p (h d) -> p h d", d=64),
                    in1=sml[j][:, smc + hb * 8:smc + hb * 8 + 8].unsqueeze(2).to_broadcast([128, 8, 64]), op=ALU.mult),
                    reads=[srcBs[hb], smlB[j]], writes=[sqB[j]])
            S.op("dve", lambda e, j=j: e.tensor_tensor(
                out=dst[j][:].rearrange("p (h d) -> p h d", d=64), in0=sq[j][:].rearrange("p (h d) -> p h d", d=64),
                in1=gqk[:, gcol:gcol + 64].unsqueeze(1).to_broadcast([128, 16, 64]), op=ALU.mult),
                reads=[sqB[j], gqkB], writes=[dstB[j]])

        for t in range(NT):
            j = t % 2
            C.load(xt[j][:], x_d[rows(t), :], xtB[j])
            C.load(yT[j][:], yT_d[t].rearrange("p (k t) -> p k t", k=16), yTB[j])
            for hb, (Pb, PbB) in enumerate(((A0, A0B), (A1, A1B))):
                for kc in range(16):
                    S.op("pe", lambda e, j=j, hb=hb, Pb=Pb, kc=kc: e.matmul(Pb[:, :], lhsT=yT[j][:, kc, :],
                                                                           rhs=wout16[:, kc, hb * 512:(hb + 1) * 512],
                                                                           start=(kc == 0), stop=(kc == 15)),
                         reads=[yTB[j], wout16B], writes=[PbB])
                S.op("dve", lambda e, j=j, hb=hb, Pb=Pb: e.tensor_tensor(out=h1[j][:, hb * 512:(hb + 1) * 512], in0=Pb[:, :],
                                                                        in1=xt[j][:, hb * 512:(hb + 1) * 512], op=ALU.add),
                     reads=[PbB, xtB[j]], writes=[h1B[j]])
            S.op("sp", lambda e, j=j, t=t: e.dma_start(out=h1_d[rows(t), :], in_=h1[j][:]), reads=[h1B[j]], writes=[outB], dma=True)
            S.op("act", lambda e, j=j: e.activation(out=junk[j][:], in_=h1[j][:], func=AF.Square, accum_out=sml[j][:, 0:1]),
                 reads=[h1B[j]], writes=[junkB[j], smlB[j]])
            S.op("act", lambda e, j=j: e.activation(out=sml[j][:, 1:2], in_=sml[j][:, 0:1], func=AF.Ln, scale=1.0 / 1024, bias=EPS),
                 reads=[smlB[j]], writes=[smlB[j]])
            S.op("act", lambda e, j=j: e.activation(out=sml[j][:, 2:3], in_=sml[j][:, 1:2], func=AF.Exp, scale=-0.5),
                 reads=[smlB[j]], writes=[smlB[j]])
            S.op("dve", lambda e, j=j: e.tensor_scalar(out=xn16[j][:], in0=h1[j][:], scalar1=sml[j][:, 2:3], scalar2=None, op0=ALU.mult),
                 reads=[h1B[j], smlB[j]], writes=[xn16B[j]])
            for k in range(8):
                S.op("pe", lambda e, j=j, k=k: e.transpose(out=PTv[:, k, :], in_=xn16[j][:, k * 128:(k + 1) * 128], identity=id16[:]),
                     reads=[xn16B[j], id16B], writes=[PTB])
            S.op("act", lambda e, j=j: e.activation(out=xnT[j][:], in_=PTv, func=AF.Copy), reads=[PTB], writes=[xnTB[j]])
            for (Pb, PbB, c0, ncol) in ((B0, B0B, 0, 512), (B1, B1B, 512, 512), (PC, PCB, 1024, 512), (PD, PDB, 1536, 68),
                                        (A0, A0B, 1604, 512), (A1, A1B, 2116, 512)):
                for kc in range(8):
                    S.op("pe", lambda e, j=j, Pb=Pb, c0=c0, ncol=ncol, kc=kc: e.matmul(
                        Pb[:, 0:ncol], lhsT=xnT[j][:, kc, :], rhs=win16[:, kc, c0:c0 + ncol], start=(kc == 0), stop=(kc == 7)),
                        reads=[xnTB[j], win16B], writes=[PbB])
            for hb, (Pb, PbB) in enumerate(((A0, A0B), (A1, A1B))):
                S.op("act", lambda e, j=j, hb=hb, Pb=Pb: e.activation(out=szt[j][:, hb * 512:(hb + 1) * 512], in_=Pb[:, :], func=AF.Silu),
                     reads=[PbB], writes=[sztB[j]])
            S.op("sp", lambda e, j=j, t=t: e.dma_start(out=sz_d[rows(t), :], in_=szt[j][:]), reads=[sztB[j]], writes=[outB], dma=True)
            S.op("act", lambda e, j=j: e.activation(out=idx[j][:, 0:256], in_=PC[:, 256:512], func=AF.Copy, scale=0.125),
                 reads=[PCB], writes=[idxB[j]])
            S.op("act", lambda e, j=j: e.activation(out=idx[j][:, 256:320], in_=PD[:, 0:64], func=AF.Copy),
                 reads=[PDB], writes=[idxB[j]])
            S.op("act", lambda e, j=j: e.activation(out=wi[j][:], in_=PD[:, 64:68], func=AF.Copy, scale=0.5),
                 reads=[PDB], writes=[wiB[j]])
            S.op("sp", lambda e, j=j, t=t: e.dma_start(out=idx_d[rows(t), :], in_=idx[j][:]), reads=[idxB[j]], writes=[outB], dma=True)
            S.op("sp", lambda e, j=j, t=t: e.dma_start(out=wi_d[rows(t), :], in_=wi[j][:]), reads=[wiB[j]], writes=[outB], dma=True)
            headnorm(j, (B0, B1), (B0B, B1B), 0, qn, qnB, -LN8, 16)
            S.op("sp", lambda e, j=j, t=t: e.dma_start(out=qn_d[rows(t), :], in_=qn[j][:]), reads=[qnB[j]], writes=[outB], dma=True)
            S.op("act", lambda e, j=j: e.activation(out=junk[j][:, 0:256], in_=PC[:, 0:256], func=AF.Square, accum_out=sml[j][:, 3:4]),
                 reads=[PCB], writes=[junkB[j], smlB[j]])
            S.op("act", lambda e, j=j: e.activation(out=sml[j][:, 4:5], in_=sml[j][:, 3:4], func=AF.Ln, scale=1.0 / 256, bias=EPS),
                 reads=[smlB[j]], writes=[smlB[j]])
            S.op("act", lambda e, j=j: e.activation(out=sml[j][:, 5:6], in_=sml[j][:, 4:5], func=AF.Exp, scale=-0.5),
                 reads=[smlB[j]], writes=[smlB[j]])
            S.op("dve", lambda e, j=j: e.tensor_scalar(out=ckvn[j][:], in0=PC[:, 0:256], scalar1=sml[j][:, 5:6], scalar2=None, op0=ALU.mult),
                 reads=[PCB, smlB[j]], writes=[ckvnB[j]])
            for k in range(2):
                S.op("pe", lambda e, j=j, k=k: e.transpose(out=PTv[:, k, :], in_=ckvn[j][:, k * 128:(k + 1) * 128], identity=id16[:]),
                     reads=[ckvnB[j], id16B], writes=[PTB])
            S.op("act", lambda e, j=j: e.activation(out=ckvT[j][:], in_=PTv[:, 0:2, :], func=AF.Copy), reads=[PTB], writes=[ckvTB[j]])
            for (Pb, PbB, w16t, w16tB, hb) in ((B0, B0B, wuk16, wuk16B, 0), (B1, B1B, wuk16, wuk16B, 1),
                                               (A0, A0B, wuv16, wuv16B, 0), (A1, A1B, wuv16, wuv16B, 1)):
                for kc in range(2):
                    S.op("pe", lambda e, j=j, Pb=Pb, w16t=w16t, hb=hb, kc=kc: e.matmul(
                        Pb[:, :], lhsT=ckvT[j][:, kc, :], rhs=w16t[:, kc, hb * 512:(hb + 1) * 512], start=(kc == 0), stop=(kc == 1)),
                        reads=[ckvTB[j], w16tB], writes=[PbB])
            headnorm(j, (B0, B1), (B0B, B1B), 64, kn, knB, 0.0, 32)
            S.op("sp", lambda e, j=j, t=t: e.dma_start(out=kn_d[rows(t), :], in_=kn[j][:]), reads=[knB[j]], writes=[outB], dma=True)
            for hb, (Pb, PbB) in enumerate(((A0, A0B), (A1, A1B))):
                S.op("act", lambda e, j=j, hb=hb, Pb=Pb: e.activation(out=vv[j][:, hb * 512:(hb + 1) * 512], in_=Pb[:, :], func=AF.Copy),
                     reads=[PbB], writes=[vvB[j]])
            S.op("sp", lambda e, j=j, t=t: e.dma_start(out=v_d[rows(t), :], in_=vv[j][:]), reads=[vvB[j]], writes=[outB], dma=True)
        S.finish(es, [outB])
    return nc


def _bf(a):
    a = np.asarray(a)
    return a if a.dtype == NPBF else a.view(NPBF)


def run_L2(inp, x_flat, y_flat, NT):
    o_w_in = inp["o_w_in"][0]
    gqk = np.tile(np.concatenate([inp["o_g_q"][0], inp["o_g_k"][0]])[None, :], (128, 1)).astype(np.float32)
    common = {
        "wout": np.ascontiguousarray(inp["e_w_out"][0]),
        "win": np.ascontiguousarray(o_w_in),
        "gn": np.ascontiguousarray(inp["o_norm"][0].reshape(8, 128).T),
        "wuk": np.ascontiguousarray(inp["o_w_uk"][0]),
        "wuv": np.ascontiguousarray(inp["o_w_uv"][0]),
        "gkv": np.ascontiguousarray(inp["o_g_kv"][0].reshape(2, 128).T),
        "gqk": gqk,
        "ident": np.eye(128, dtype=np.float32),
    }
    n = NT * 128
    in_maps = []
    for core in range(8):
        ys = y_flat[core * n:(core + 1) * n]
        yT = ys.reshape(NT, 128, 16, 128).transpose(0, 3, 2, 1)
        m = dict(common)
        m["x"] = np.ascontiguousarray(x_flat[core * n:(core + 1) * n])
        m["yT"] = np.ascontiguousarray(yT).reshape(NT, 128, 16 * 128)
        in_maps.append(m)
    nc = build_L2(NT)
    res = run_bass_kernel_spmd(nc, in_maps, core_ids=list(range(8)))
    out = {}
    for k in ("h1", "wi"):
        out[k] = np.concatenate([np.asarray(res.results[c][k]) for c in range(8)], axis=0)
    for k in ("qn", "kn", "v", "idx", "sz"):
        out[k] = np.concatenate([_bf(res.results[c][k]) for c in range(8)], axis=0)
    return out


NBIS = 18
NEG = -30000.0


def build_L3(S_len):
    NKT = S_len // 128
    NS = NKT // 4
    nc = bass.Bass("TRN2", target_bir_lowering=False)
    es = ExitStack()
    with es:
        C = Ctx(nc, es)
        S = C.S
        kiT_d = C.dram_in("kiT", [64, S_len], BF16)
        KT_d = C.dram_in("KT", [NKT, 65, 2048], BF16)
        VA_d = C.dram_in("VA", [NKT, 128, 1040], BF16)
        qiT_d = C.dram_in("qiT", [NS, 64, 512], BF16)
        QT_d = C.dram_in("QT", [NS, 65, 2048], BF16)
        wi_d = C.dram_in("wi", [NS, 128, 4])
        sz_d = C.dram_in("sz", [NS, 128, 1024], BF16)
        h1_d = C.dram_in("h1", [NS, 128, 1024])
        tp_d = C.dram_in("tpos", [NS, 128, 2])
        nb_d = C.dram_in("nbraw", [128, 5 * 16 * 128])
        cb_d = C.dram_in("cb", [128, 16])
        wo_d = C.dram_in("wo", [1024, 1024])
        cst_d = C.dram_in("cst", [128, 128 + 512])
        out_d = C.dram_out("out", [NS, 128, 1024])
        outB = Buf("out")

        cst, cstB = C.sb([128, 640], F32, "cst")
        C.load(cst[:], cst_d, cstB)
        kpos = cst[:, 128:640]
        id4, id4B = C.sb([128, 4, 128], BF16, "id4")
        for a in range(4):
            S.op("dve", lambda e, a=a: e.tensor_copy(out=id4[:, a, :], in_=cst[:, 0:128]), reads=[cstB], writes=[id4B])
        id16 = id4[:, 0, :]
        half, halfB = C.sb([128, 1], F32, "half")
        S.op("pool", lambda e: e.memset(half[:], 0.5), writes=[halfB])
        one16, one16B = C.sb([128, 1], BF16, "one16")
        S.op("pool", lambda e: e.memset(one16[:], 1.0), writes=[one16B])
        cb, cbB = C.sb([128, 16], F32, "cb")
        C.load(cb[:], cb_d, cbB)
        nb, nbB = C.sb([128, 5, 16, 128], BF16, "nb")
        nbst, nbstB = C.sb([128, 4, 128], F32, "nbst")
        for u in range(5):
            for g4 in range(4):
                C.load(nbst[:], nb_d[:, u * 2048 + g4 * 512:u * 2048 + (g4 + 1) * 512].rearrange("p (h t) -> p h t", h=4), nbstB)
                S.op("dve", lambda e, u=u, g4=g4: e.tensor_tensor(out=nb[:, u, 4 * g4:4 * g4 + 4, :], in0=nbst[:],
                                                                 in1=cb[:, 4 * g4:4 * g4 + 4].unsqueeze(2).to_broadcast([128, 4, 128]),
                                                                 op=ALU.subtract),
                     reads=[nbstB, cbB], writes=[nbB])

        def dbl(shape, dt, name, n=2):
            a = [C.sb(shape, dt, name + str(i)) for i in range(n)]
            return [t for t, _ in a], [b for _, b in a]
        NKMAX = S_len
        sc16, sc16B = C.sb([128, NKMAX], BF16, "sc16")
        T1, T1B = C.sb([128, NKMAX], mybir.dt.uint8, "T1")
        Mb, MbB = dbl([128, NKMAX], BF16, "Mb")
        wst, wstB = dbl([128, 512], F32, "wst")
        woc, wocB = dbl([128, 512], BF16, "woc")
        kib, kibB = dbl([64, 512], BF16, "kib")
        qiT, qiTB = dbl([64, 512], BF16, "qiT")
        wi, wiB = dbl([128, 4], F32, "wi")
        tp, tpB = dbl([128, 2], F32, "tp")
        rl, rlB = dbl([128, 2, 512], F32, "rl", 1)
        rl = rl * 2
        rlB = rlB * 2
        acc, accB = dbl([128, 512], F32, "acc")
        bs, bsB = dbl([128, 16], F32, "bs")
        QT, QTB = dbl([65, 2048], BF16, "QT", 1)
        QT, QTB = QT * 2, QTB * 2
        KT, KTB = dbl([65, 2048], BF16, "KT", 2)
        VA, VAB = dbl([128, 1040], BF16, "VA", 2)
        pt, ptB = dbl([128, 4, 128], BF16, "pt", 3)
        def sgl(shape, dt, name):
            a, b = dbl(shape, dt, name, 1)
            return a * 2, b * 2
        szt, sztB = sgl([128, 1024], BF16, "szt")
        h1t, h1tB = sgl([128, 1024], F32, "h1t")
        rd, rdB = sgl([128, 16], F32, "rd")
        yf, yfB = sgl([128, 1024], F32, "yf")
        y16, y16B = sgl([128, 1024], BF16, "y16")
        yT, yTB = sgl([128, 8, 128], BF16, "yT")
        ot, otB = sgl([128, 1024], F32, "ot")

        SC = [C.bank("SC%d" % i) for i in range(2)]
        SCB = [Buf() for _ in range(2)]
        LB = [C.bank("L%d" % i) for i in range(2)]
        LBB = [Buf() for _ in range(2)]
        OB = [C.bank("O%d" % i) for i in range(3)]
        OBB = [Buf() for _ in range(3)]
        XB = C.bank("X")
        XBv = XB[:, :].bitcast(BF16).rearrange("p (a b) -> p a b", a=8)
        XBB = Buf()
        def ocol(h):
            return (h // 7, (h % 7) * 65)

        cnt = {"kv": 0, "pt": 0, "lb": 0}

        def phase_AB(r):
            j = r % 2
            nblk = r + 1
            nk = nblk * 512
            C.load(qiT[j][:], qiT_d[r], qiTB[j])
            C.load(wi[j][:], wi_d[r], wiB[j])
            C.load(tp[j][:], tp_d[r], tpB[j])
            for blk in range(nblk):
                kj = blk % 2
                C.load(kib[kj][:], kiT_d[:, blk * 512:(blk + 1) * 512], kibB[kj])
                for pr in range(2):
                    for hh in range(2):
                        S.op("pe", lambda e, j=j, kj=kj, pr=pr, hh=hh: e.matmul(
                            SC[hh][:, :], lhsT=qiT[j][:, (pr * 2 + hh) * 128:(pr * 2 + hh + 1) * 128], rhs=kib[kj][:, :],
                            start=True, stop=True), reads=[qiTB[j], kibB[kj]], writes=[SCB[hh]])
                        S.op("act", lambda e, kj=kj, hh=hh: e.activation(out=rl[kj][:, hh, :], in_=SC[hh][:, :], func=AF.Relu),
                             reads=[SCB[hh]], writes=[rlB[kj]])
                    for hh in range(2):
                        h = pr * 2 + hh
                        last = (h == 3)
                        dst = sc16[:, blk * 512:(blk + 1) * 512] if (last and blk < nblk - 1) else acc[kj][:]
                        dstB = sc16B if (last and blk < nblk - 1) else accB[kj]
                        if h == 0:
                            S.op("dve", lambda e, j=j, kj=kj, hh=hh, h=h: e.tensor_scalar(
                                out=acc[kj][:], in0=rl[kj][:, hh, :], scalar1=wi[j][:, h:h + 1], scalar2=None, op0=ALU.mult),
                                reads=[rlB[kj], wiB[j]], writes=[accB[kj]])
                        else:
                            S.op("dve", lambda e, j=j, kj=kj, hh=hh, h=h, dst=dst: e.scalar_tensor_tensor(
                                out=dst, in0=rl[kj][:, hh, :], scalar=wi[j][:, h:h + 1], in1=acc[kj][:], op0=ALU.mult, op1=ALU.add),
                                reads=[rlB[kj], wiB[j], accB[kj]], writes=[dstB])
                if blk == nblk - 1:
                    S.op("dve", lambda e, j=j, r=r: e.tensor_scalar(out=bs[j][:, 0:1], in0=tp[j][:, 0:1], scalar1=float(-512 * r),
                                                                   scalar2=None, op0=ALU.add),
                         reads=[tpB[j]], writes=[bsB[j]])
                    S.op("dve", lambda e, j=j, kj=kj: e.tensor_scalar(out=rl[kj][:, 0, :], in0=kpos, scalar1=bs[j][:, 0:1], scalar2=-1e30,
                                                                     op0=ALU.is_gt, op1=ALU.mult),
                         reads=[cstB, bsB[j]], writes=[rlB[kj]])
                    S.op("dve", lambda e, kj=kj, blk=blk: e.tensor_tensor(out=sc16[:, blk * 512:(blk + 1) * 512], in0=acc[kj][:],
                                                                         in1=rl[kj][:, 0, :], op=ALU.add),
                         reads=[accB[kj], rlB[kj]], writes=[sc16B])
            sc = sc16[:, 0:nk]
            t1 = T1[:, 0:nk]
            b = bs[j]
            bB = bsB[j]
            S.op("dve", lambda e: e.tensor_reduce(out=b[:, 2:3], in_=sc, axis=AX.X, op=ALU.max), reads=[sc16B], writes=[bB])
            S.op("dve", lambda e: e.tensor_scalar(out=b[:, 2:3], in0=b[:, 2:3], scalar1=1.0, scalar2=None, op0=ALU.add),
                 reads=[bB], writes=[bB])
            kjl = (nblk - 1) % 2
            S.op("dve", lambda e: e.tensor_reduce(out=b[:, 1:2], in_=acc[kjl][:], axis=AX.X, op=ALU.min), reads=[accB[kjl]], writes=[bB])
            if nblk > 1:
                S.op("dve", lambda e: e.tensor_reduce(out=b[:, 7:8], in_=sc16[:, 0:nk - 512], axis=AX.X, op=ALU.min),
                     reads=[sc16B], writes=[bB])
                S.op("dve", lambda e: e.tensor_tensor(out=b[:, 1:2], in0=b[:, 1:2], in1=b[:, 7:8], op=ALU.min), reads=[bB], writes=[bB])
            S.op("dve", lambda e: e.tensor_scalar(out=b[:, 1:2], in0=b[:, 1:2], scalar1=-1.0, scalar2=None, op0=ALU.add),
                 reads=[bB], writes=[bB])
            S.op("dve", lambda e: e.memset(b[:, 6:7], 0.0), writes=[bB])
            for it in range(NBIS):
                S.op("dve", lambda e: e.scalar_tensor_tensor(out=b[:, 3:4], in0=b[:, 1:2], scalar=b[:, 2:3], in1=half[:],
                                                             op0=ALU.add, op1=ALU.mult), reads=[bB, halfB], writes=[bB])
                S.op("dve", lambda e: e.tensor_scalar(out=t1, in0=sc, scalar1=b[:, 3:4], scalar2=None, op0=ALU.is_ge, op1=ALU.add,
                                                      accum_out=b[:, 4:5]), reads=[sc16B, bB], writes=[T1B, bB])
                S.op("dve", lambda e, j=j: e.tensor_tensor(out=b[:, 5:6], in0=b[:, 4:5], in1=tp[j][:, 1:2], op=ALU.is_ge),
                     reads=[bB, tpB[j]], writes=[bB])
                S.op("dve", lambda e: e.tensor_tensor(out=b[:, 7:8], in0=b[:, 3:4], in1=b[:, 1:2], op=ALU.subtract), reads=[bB], writes=[bB])
                S.op("dve", lambda e: e.scalar_tensor_tensor(out=b[:, 1:2], in0=b[:, 7:8], scalar=b[:, 5:6], in1=b[:, 1:2],
                                                             op0=ALU.mult, op1=ALU.add), reads=[bB], writes=[bB])
                S.op("dve", lambda e: e.tensor_tensor(out=b[:, 7:8], in0=b[:, 2:3], in1=b[:, 3:4], op=ALU.subtract), reads=[bB], writes=[bB])
                S.op("dve", lambda e: e.scalar_tensor_tensor(out=b[:, 2:3], in0=b[:, 7:8], scalar=b[:, 5:6], in1=b[:, 3:4],
                                                             op0=ALU.mult, op1=ALU.add), reads=[bB], writes=[bB])
                S.op("dve", lambda e: e.tensor_tensor(out=b[:, 7:8], in0=b[:, 6:7], in1=b[:, 4:5], op=ALU.subtract), reads=[bB], writes=[bB])
                S.op("dve", lambda e: e.scalar_tensor_tensor(out=b[:, 6:7], in0=b[:, 7:8], scalar=b[:, 5:6], in1=b[:, 4:5],
                                                             op0=ALU.mult, op1=ALU.add), reads=[bB], writes=[bB])
            mb = Mb[j][:, 0:nk]
            S.op("dve", lambda e, j=j: e.tensor_tensor(out=b[:, 9:10], in0=tp[j][:, 1:2], in1=b[:, 6:7], op=ALU.subtract),
                 reads=[bB, tpB[j]], writes=[bB])
            S.op("dve", lambda e: e.tensor_scalar(out=mb, in0=sc, scalar1=b[:, 2:3], scalar2=None, op0=ALU.is_ge),
                 reads=[sc16B, bB], writes=[MbB[j]])
            S.op("dve", lambda e: e.tensor_scalar(out=t1, in0=sc, scalar1=b[:, 1:2], scalar2=None, op0=ALU.is_ge),
                 reads=[sc16B, bB], writes=[T1B])
            S.op("dve", lambda e: e.tensor_tensor(out=t1, in0=t1, in1=mb, op=ALU.subtract), reads=[T1B, MbB[j]], writes=[T1B])
            S.op("dve", lambda e: e.tensor_tensor_scan(out=sc, data0=one16[:, 0:1].to_broadcast([128, nk]), data1=t1, initial=0.0,
                                                       op0=ALU.mult, op1=ALU.add), reads=[T1B, one16B, sc16B], writes=[sc16B])
            S.op("dve", lambda e: e.scalar_tensor_tensor(out=t1, in0=sc, scalar=b[:, 9:10], in1=t1, op0=ALU.is_le, op1=ALU.mult),
                 reads=[sc16B, bB, T1B], writes=[T1B])
            S.op("dve", lambda e: e.tensor_tensor(out=mb, in0=mb, in1=t1, op=ALU.add), reads=[MbB[j], T1B], writes=[MbB[j]])
            S.op("dve", lambda e: e.tensor_scalar(out=mb, in0=mb, scalar1=-1.0, scalar2=-NEG, op0=ALU.add, op1=ALU.mult),
                 reads=[MbB[j]], writes=[MbB[j]])

        def phase_CD(r):
            j = r % 2
            nkt = 4 * r + 4
            C.load(QT[j][:], QT_d[r], QTB[j])
            C.load(szt[j][:], sz_d[r], sztB[j])
            C.load(h1t[j][:], h1_d[r], h1tB[j])
            ostart = [True, True, True]
            for jt in range(nkt):
                kv = cnt["kv"] % 2
                cnt["kv"] += 1
                C.load(KT[kv][:], KT_d[jt], KTB[kv])
                C.load(VA[kv][:], VA_d[jt], VAB[kv])
                u = jt - (4 * r - 1)
                for g in range(4):
                    lb = cnt["lb"] % 2
                    cnt["lb"] += 1
                    Lv = LB[lb][:, :].rearrange("p (a b) -> p a b", a=4)
                    for hh in range(4):
                        h = 4 * g + hh
                        S.op("pe", lambda e, j=j, kv=kv, h=h, hh=hh, Lv=Lv: e.matmul(
                            Lv[:, hh, :], lhsT=KT[kv][:, h * 128:(h + 1) * 128], rhs=QT[j][:, h * 128:(h + 1) * 128],
                            start=(hh == 0), stop=False), reads=[KTB[kv], QTB[j]], writes=[LBB[lb]])
                    S.op("pe", lambda e, j=j, jt=jt, lb=lb, u=u: e.matmul(
                        LB[lb][:, :], lhsT=Mb[j][:, jt * 128:(jt + 1) * 128], rhs=id4[:].rearrange("p a b -> p (a b)"),
                        start=False, stop=(u < 0)), reads=[MbB[j], id4B], writes=[LBB[lb]])
                    if u >= 0:
                        S.op("pe", lambda e, lb=lb, u=u, g=g: e.matmul(
                            LB[lb][:, :], lhsT=id16, rhs=nb[:, u, 4 * g:4 * g + 4, :].rearrange("p a b -> p (a b)"),
                            start=False, stop=True), reads=[nbB, id4B], writes=[LBB[lb]])
                    pi = cnt["pt"] % 3
                    cnt["pt"] += 1
                    S.op("act", lambda e, lb=lb, pi=pi: e.activation(out=pt[pi][:].rearrange("p a b -> p (a b)"), in_=LB[lb][:, :], func=AF.Exp),
                         reads=[LBB[lb]], writes=[ptB[pi]])
                    for hh in range(4):
                        h = 4 * g + hh
                        ob, oc = ocol(h)
                        st = ostart[ob]
                        ostart[ob] = False
                        S.op("pe", lambda e, kv=kv, pi=pi, hh=hh, h=h, ob=ob, oc=oc, st=st, jt=jt: e.matmul(
                            OB[ob][:, oc:oc + 65], lhsT=pt[pi][:, hh, :], rhs=VA[kv][:, h * 65:(h + 1) * 65],
                            start=st, stop=(jt == nkt - 1)), reads=[ptB[pi], VAB[kv]], writes=[OBB[ob]])
            for ob, nh in ((0, 7), (1, 7), (2, 2)):
                Ov = OB[ob][:, 0:nh * 65].rearrange("p (h d) -> p h d", d=65)
                S.op("dve", lambda e, j=j, ob=ob, nh=nh, Ov=Ov: e.reciprocal(out=rd[j][:, ob * 7:ob * 7 + nh], in_=Ov[:, :, 64]),
                     reads=[OBB[ob]], writes=[rdB[j]])
                S.op("dve", lambda e, j=j, ob=ob, nh=nh, Ov=Ov: e.tensor_tensor(
                    out=yf[j][:, ob * 448:ob * 448 + nh * 64].rearrange("p (h d) -> p h d", d=64), in0=Ov[:, :, 0:64],
                    in1=rd[j][:, ob * 7:ob * 7 + nh].unsqueeze(2).to_broadcast([128, nh, 64]), op=ALU.mult),
                    reads=[OBB[ob], rdB[j]], writes=[yfB[j]])
            S.op("dve", lambda e, j=j: e.tensor_tensor(out=y16[j][:], in0=yf[j][:], in1=szt[j][:], op=ALU.mult),
                 reads=[yfB[j], sztB[j]], writes=[y16B[j]])
            for k in range(8):
                S.op("pe", lambda e, j=j, k=k: e.transpose(out=XBv[:, k, :], in_=y16[j][:, k * 128:(k + 1) * 128], identity=id16),
                     reads=[y16B[j], id4B], writes=[XBB])
            S.op("act", lambda e, j=j: e.activation(out=yT[j][:], in_=XBv, func=AF.Copy), reads=[XBB], writes=[yTB[j]])
            for hb in range(2):
                lb = cnt["lb"] % 2
                cnt["lb"] += 1
                for kc in range(8):
                    wj = kc % 2
                    C.load(wst[wj][:], wo_d[kc * 128:(kc + 1) * 128, hb * 512:(hb + 1) * 512], wstB[wj])
                    S.op("act", lambda e, wj=wj: e.activation(out=woc[wj][:], in_=wst[wj][:], func=AF.Copy),
                         reads=[wstB[wj]], writes=[wocB[wj]])
                    S.op("pe", lambda e, j=j, lb=lb, kc=kc, wj=wj: e.matmul(LB[lb][:, :], lhsT=yT[j][:, kc, :], rhs=woc[wj][:],
                                                                           start=(kc == 0), stop=(kc == 7)),
                         reads=[yTB[j], wocB[wj]], writes=[LBB[lb]])
                S.op("dve", lambda e, j=j, lb=lb, hb=hb: e.tensor_tensor(out=ot[j][:, hb * 512:(hb + 1) * 512], in0=LB[lb][:, :],
                                                                        in1=h1t[j][:, hb * 512:(hb + 1) * 512], op=ALU.add),
                     reads=[LBB[lb], h1tB[j]], writes=[otB[j]])
            S.op("sp", lambda e, j=j, r=r: e.dma_start(out=out_d[r], in_=ot[j][:]), reads=[otB[j]], writes=[outB], dma=True)

        phase_AB(0)
        for r in range(NS):
            if r + 1 < NS:
                phase_AB(r + 1)
            phase_CD(r)
        S.finish(es, [outB])
    return nc


def t5_bucket_np(rel):
    rel = np.maximum(rel, 0)
    max_exact = 16
    rel_f = np.maximum(rel, 1).astype(np.float32)
    large = max_exact + (np.log(rel_f / max_exact) / math.log(128 / max_exact) * (32 - max_exact)).astype(np.int32)
    large = np.minimum(large, 31)
    return np.where(rel < max_exact, rel, large)


def run_L3(inp, l2, B, S_len):
    NKT = S_len // 128
    NS = NKT // 4
    rel_bias = inp["rel_bias"]
    cbv = rel_bias[31].astype(np.float32)
    qn = l2["qn"].reshape(B, S_len, 16, 64)
    kn = l2["kn"].reshape(B, S_len, 16, 64)
    v = l2["v"].reshape(B, S_len, 16, 64)
    idx = l2["idx"].reshape(B, S_len, 320)
    sz = l2["sz"].reshape(B, NKT, 128, 1024)
    h1 = l2["h1"].reshape(B, NKT, 128, 1024)
    wi = l2["wi"].reshape(B, NKT, 128, 4)
    KT, VA, kiT = [], [], []
    for b in range(B):
        k_ = np.ones((NKT, 65, 16, 128), NPBF)
        k_[:, 0:64] = kn[b].reshape(NKT, 128, 16, 64).transpose(0, 3, 2, 1)
        KT.append(k_.reshape(NKT, 65, 2048))
        v_ = np.ones((NKT, 128, 16, 65), NPBF)
        v_[..., 0:64] = v[b].reshape(NKT, 128, 16, 64)
        VA.append(v_.reshape(NKT, 128, 1040))
        kiT.append(np.ascontiguousarray(idx[b, :, 256:320].T))
    QTa = np.zeros((B, NKT, 65, 16, 128), NPBF)
    QTa[:, :, 0:64] = qn.reshape(B, NKT, 128, 16, 64).transpose(0, 1, 4, 3, 2)
    QTa[:, :, 64] = cbv.astype(NPBF)[None, None, :, None]
    qiTa = idx[:, :, 0:256].reshape(B, NKT, 128, 4, 64).transpose(0, 1, 4, 3, 2)
    s_ = np.arange(128)
    ident = np.eye(128, dtype=np.float32)
    cst = np.concatenate([ident, np.tile(np.arange(512, dtype=np.float32)[None, :], (128, 1))], axis=1)
    wo = np.ascontiguousarray(inp["o_w_out"][0])
    cb = np.tile(cbv[None, :], (128, 1)).astype(np.float32)
    in_maps = []
    for core in range(8):
        b, m = core // 4, core % 4
        b = min(b, B - 1)
        tiles = [4 * r + m for r in range(NS)]
        nbraw = np.zeros((128, 5, 16, 128), np.float32)
        for u in range(5):
            delta = m + 1 - u
            rel = (s_[None, :] + 128 * delta) - s_[:, None]
            bk = t5_bucket_np(rel)
            nbraw[:, u] = rel_bias[bk].transpose(0, 2, 1)
        tpos = np.zeros((NS, 128, 2), np.float32)
        for r, i in enumerate(tiles):
            t = i * 128 + s_
            tpos[r, :, 0] = t
            tpos[r, :, 1] = np.minimum(256, t + 1)
        in_maps.append({
            "kiT": kiT[b], "KT": KT[b], "VA": VA[b],
            "qiT": np.ascontiguousarray(qiTa[b, tiles]).reshape(NS, 64, 512),
            "QT": np.ascontiguousarray(QTa[b, tiles]).reshape(NS, 65, 2048),
            "wi": np.ascontiguousarray(wi[b, tiles]), "sz": np.ascontiguousarray(sz[b, tiles]),
            "h1": np.ascontiguousarray(h1[b, tiles]), "tpos": tpos,
            "nbraw": nbraw.reshape(128, 5 * 16 * 128), "cb": cb, "wo": wo, "cst": cst,
        })
    nc = build_L3(S_len)
    res = run_bass_kernel_spmd(nc, in_maps, core_ids=list(range(8)))
    out = np.zeros((B, NKT, 128, 1024), np.float32)
    for core in range(8):
        b, m = core // 4, core % 4
        if b >= B:
            continue
        o = np.asarray(res.results[core]["out"])
        for r in range(NS):
            out[b, 4 * r + m] = o[r]
    return out.reshape(B, S_len, 1024)


def kernel(x, e_norm, e_w_in, e_conv, e_b_if, e_g_head, e_w_pool, e_s_pool, e_w_out,
           o_norm, o_w_in, o_g_kv, o_w_uk, o_w_uv, o_g_q, o_g_k, o_w_out, rel_bias):
    inp = {k: np.asarray(v) for k, v in dict(
        x=x, e_norm=e_norm, e_w_in=e_w_in, e_conv=e_conv, e_b_if=e_b_if, e_g_head=e_g_head, e_w_pool=e_w_pool,
        e_s_pool=e_s_pool, e_w_out=e_w_out, o_norm=o_norm, o_w_in=o_w_in, o_g_kv=o_g_kv, o_w_uk=o_w_uk,
        o_w_uv=o_w_uv, o_g_q=o_g_q, o_g_k=o_g_k, o_w_out=o_w_out, rel_bias=rel_bias).items()}
    B, S_len, D = inp["x"].shape
    y = run_L1(inp, S_len)
    NT = B * S_len // 128 // 8
    l2 = run_L2(inp, inp["x"].reshape(B * S_len, D), y.reshape(B * S_len, 2048), NT)
    out = run_L3(inp, l2, B, S_len)
    return out.astype(np.float32)
```
